# Optimizing a Trainium2 kernel written in Bass

```python
import math
import jax, jax.numpy as jnp
from jax import lax
import numpy as np

D_MODEL = 1024
BATCH = 2
SEQ = 8192
DEPTH = 2

HEAD_DIM = 64
N_HEADS = 8
N_KV_HEADS = 2
GQA = N_HEADS // N_KV_HEADS
ATTN_WIDTH = N_HEADS * HEAD_DIM
KV_WIDTH = N_KV_HEADS * HEAD_DIM
WINDOW = 128
ATTN_BLOCK = 128
SSM_WIDTH = D_MODEL - ATTN_WIDTH
SSM_GROUP_DIM = 16
SSM_GROUPS = SSM_WIDTH // SSM_GROUP_DIM
SSM_STATE = 64
DT_MIN = 1e-3
DT_MAX = 1e-1
IN_WIDTH = ATTN_WIDTH + 2 * KV_WIDTH + SSM_WIDTH
D_FF = 3584
N_EXPERTS = 8
TOP_K = 2
MOE_BLOCK = 256
N_DENSE = (DEPTH + 1) // 2
N_MOE = DEPTH // 2
NORM_EPS = 1e-6
NEG_INF = -1e30

kernel_name = "hymba_swa_sink_s5_moe"


def _rms(x, g):
    xf = x.astype(jnp.float32)
    y = xf * lax.rsqrt(jnp.mean(xf * xf, axis=-1, keepdims=True) + NORM_EPS)
    return y * g.astype(jnp.float32)


def _swa_sink_attention(q, k, v, sinks):
    bsz, seq = q.shape[0], q.shape[1]
    nb = seq // ATTN_BLOCK
    qb = q.reshape(bsz, nb, ATTN_BLOCK, N_KV_HEADS, GQA, HEAD_DIM)
    kb = k.reshape(bsz, nb, ATTN_BLOCK, N_KV_HEADS, HEAD_DIM)
    vb = v.reshape(bsz, nb, ATTN_BLOCK, N_KV_HEADS, HEAD_DIM)

    def with_prev(t):
        prev = jnp.concatenate([jnp.zeros_like(t[:, :1]), t[:, :-1]], axis=1)
        return jnp.concatenate([prev, t], axis=2)

    kk, vv = with_prev(kb), with_prev(vb)
    s = jnp.einsum('bnqkgd,bnskd->bnkgqs', qb, kk) * (HEAD_DIM ** -0.5)
    qi = jnp.arange(ATTN_BLOCK)[:, None]
    sj = jnp.arange(2 * ATTN_BLOCK)[None, :]
    delta = qi + ATTN_BLOCK - sj
    band = (delta >= 0) & (delta < WINDOW)
    not_first = (jnp.arange(nb) > 0)[:, None, None]
    valid = band[None] & (not_first | (sj >= ATTN_BLOCK)[None])
    s = jnp.where(valid[None, :, None, None], s, NEG_INF)
    sink = jnp.broadcast_to(
        sinks.astype(jnp.float32).reshape(1, 1, N_KV_HEADS, GQA, 1, 1), s.shape[:-1] + (1,))
    p = jax.nn.softmax(jnp.concatenate([s, sink], axis=-1), axis=-1)[..., :-1]
    o = jnp.einsum('bnkgqs,bnskd->bnqkgd', p, vv)
    return o.reshape(bsz, seq, ATTN_WIDTH)


def _s5_mixer(u, lam_re, lam_im, log_dt, b_re, b_im, c_re, c_im, d_skip, w_glu):
    f32 = jnp.float32
    bsz, seq = u.shape[0], u.shape[1]
    ug = u.astype(f32).reshape(bsz, seq, SSM_GROUPS, SSM_GROUP_DIM)
    lr, li = lam_re.astype(f32), lam_im.astype(f32)
    dt = jnp.exp(log_dt.astype(f32))[:, None]
    mag = jnp.exp(lr * dt)
    ang = li * dt
    ab_re, ab_im = mag * jnp.cos(ang), mag * jnp.sin(ang)
    nr = ab_re - 1.0
    den = lr * lr + li * li
    f_re = (nr * lr + ab_im * li) / den
    f_im = (ab_im * lr - nr * li) / den
    br, bi = b_re.astype(f32), b_im.astype(f32)
    bb_re = f_re[..., None] * br - f_im[..., None] * bi
    bb_im = f_re[..., None] * bi + f_im[..., None] * br
    bu_re = jnp.einsum('blgc,gnc->blgn', ug, bb_re)
    bu_im = jnp.einsum('blgc,gnc->blgn', ug, bb_im)
    a_re = jnp.broadcast_to(ab_re, bu_re.shape)
    a_im = jnp.broadcast_to(ab_im, bu_im.shape)

    def combine(e1, e2):
        a1r, a1i, b1r, b1i = e1
        a2r, a2i, b2r, b2i = e2
        return (a2r * a1r - a2i * a1i,
                a2r * a1i + a2i * a1r,
                a2r * b1r - a2i * b1i + b2r,
                a2r * b1i + a2i * b1r + b2i)

    _, _, s_re, s_im = lax.associative_scan(combine, (a_re, a_im, bu_re, bu_im), axis=1)
    y = (jnp.einsum('blgn,gcn->blgc', s_re, c_re.astype(f32))
         - jnp.einsum('blgn,gcn->blgc', s_im, c_im.astype(f32))
         + d_skip.astype(f32).reshape(SSM_GROUPS, SSM_GROUP_DIM) * ug)
    y = jax.nn.gelu(y.reshape(bsz, seq, SSM_WIDTH))
    return y * jax.nn.sigmoid(y @ w_glu.astype(f32))


def _swiglu(h, wg, wu, wd):
    return (jax.nn.silu(h @ wg) * (h @ wu)) @ wd


def _moe_swiglu(x2d, w_router, wg, wu, wd):
    n_tok = x2d.shape[0]
    logits = (x2d @ w_router).astype(jnp.float32)
    top_v, top_i = lax.top_k(logits, TOP_K)
    gates = jax.nn.softmax(top_v, axis=-1).astype(x2d.dtype)
    n_assign = n_tok * TOP_K
    flat_e = top_i.reshape(-1)
    flat_tok = jnp.repeat(jnp.arange(n_tok, dtype=jnp.int32), TOP_K)
    flat_g = gates.reshape(-1)
    order = jnp.argsort(flat_e)
    se = flat_e[order]
    counts = jnp.bincount(flat_e, length=N_EXPERTS)
    start = jnp.cumsum(counts) - counts
    padded = ((counts + MOE_BLOCK - 1) // MOE_BLOCK) * MOE_BLOCK
    pend = jnp.cumsum(padded)
    pstart = pend - padded
    dest = pstart[se] + jnp.arange(n_assign) - start[se]
    nblk = -(-n_assign // MOE_BLOCK) + N_EXPERTS
    cap = nblk * MOE_BLOCK
    buf_tok = jnp.zeros((cap,), jnp.int32).at[dest].set(flat_tok[order])
    buf_gate = jnp.zeros((cap,), x2d.dtype).at[dest].set(flat_g[order])
    block_e = jnp.clip(jnp.searchsorted(pend, jnp.arange(nblk) * MOE_BLOCK, side='right'),
                       0, N_EXPERTS - 1)

    def run_block(args):
        tok, g, e = args
        xb = x2d[tok]
        return _swiglu(xb, wg[e], wu[e], wd[e]) * g[:, None]

    y = lax.map(run_block, (buf_tok.reshape(nblk, MOE_BLOCK),
                            buf_gate.reshape(nblk, MOE_BLOCK), block_e))
    return jnp.zeros_like(x2d).at[buf_tok].add(y.reshape(cap, D_MODEL))


def setup_inputs(seed: int = 0) -> dict:
    key = jax.random.key(seed)
    ks = iter(jax.random.split(key, 32))
    nrm = lambda shape, scale: jax.random.normal(next(ks), shape, jnp.float32) * scale
    gain = lambda shape: 1.0 + nrm(shape, 0.02)
    n_idx = jnp.arange(SSM_STATE, dtype=jnp.float32)[None, None, :]
    return {
        "x": nrm((BATCH, SEQ, D_MODEL), 1.0),
        "attn_norm_g": gain((DEPTH, D_MODEL)),
        "w_in": nrm((DEPTH, D_MODEL, IN_WIDTH), D_MODEL ** -0.5),
        "q_norm_g": gain((DEPTH, HEAD_DIM)),
        "k_norm_g": gain((DEPTH, HEAD_DIM)),
        "sinks": nrm((DEPTH, N_HEADS), 0.5),
        "lam_re": -0.5 + nrm((DEPTH, SSM_GROUPS, SSM_STATE), 0.01),
        "lam_im": math.pi * n_idx + nrm((DEPTH, SSM_GROUPS, SSM_STATE), 0.01),
        "log_dt": jax.random.uniform(next(ks), (DEPTH, SSM_GROUPS), jnp.float32,
                                     math.log(DT_MIN), math.log(DT_MAX)),
        "b_re": nrm((DEPTH, SSM_GROUPS, SSM_STATE, SSM_GROUP_DIM), (2 * SSM_GROUP_DIM) ** -0.5),
        "b_im": nrm((DEPTH, SSM_GROUPS, SSM_STATE, SSM_GROUP_DIM), (2 * SSM_GROUP_DIM) ** -0.5),
        "c_re": nrm((DEPTH, SSM_GROUPS, SSM_GROUP_DIM, SSM_STATE), (2 * SSM_STATE) ** -0.5),
        "c_im": nrm((DEPTH, SSM_GROUPS, SSM_GROUP_DIM, SSM_STATE), (2 * SSM_STATE) ** -0.5),
        "d_skip": nrm((DEPTH, SSM_WIDTH), 1.0),
        "w_glu": nrm((DEPTH, SSM_WIDTH, SSM_WIDTH), SSM_WIDTH ** -0.5),
        "attn_out_g": gain((DEPTH, ATTN_WIDTH)),
        "ssm_out_g": gain((DEPTH, SSM_WIDTH)),
        "w_o": nrm((DEPTH, D_MODEL, D_MODEL), D_MODEL ** -0.5),
        "ffn_norm_g": gain((DEPTH, D_MODEL)),
        "dense_wg": nrm((N_DENSE, D_MODEL, D_FF), D_MODEL ** -0.5),
        "dense_wu": nrm((N_DENSE, D_MODEL, D_FF), D_MODEL ** -0.5),
        "dense_wd": nrm((N_DENSE, D_FF, D_MODEL), D_FF ** -0.5),
        "router_w": nrm((N_MOE, D_MODEL, N_EXPERTS), D_MODEL ** -0.5),
        "moe_wg": nrm((N_MOE, N_EXPERTS, D_MODEL, D_FF), D_MODEL ** -0.5),
        "moe_wu": nrm((N_MOE, N_EXPERTS, D_MODEL, D_FF), D_MODEL ** -0.5),
        "moe_wd": nrm((N_MOE, N_EXPERTS, D_FF, D_MODEL), D_FF ** -0.5),
    }


def reference(x, attn_norm_g, w_in, q_norm_g, k_norm_g, sinks, lam_re, lam_im, log_dt,
              b_re, b_im, c_re, c_im, d_skip, w_glu, attn_out_g, ssm_out_g, w_o,
              ffn_norm_g, dense_wg, dense_wu, dense_wd, router_w, moe_wg, moe_wu, moe_wd):
    bsz, seq = x.shape[0], x.shape[1]
    for l in range(DEPTH):
        hn = _rms(x, attn_norm_g[l]).astype(x.dtype)
        proj = hn @ w_in[l]
        q = proj[..., :ATTN_WIDTH].reshape(bsz, seq, N_HEADS, HEAD_DIM)
        k = proj[..., ATTN_WIDTH:ATTN_WIDTH + KV_WIDTH].reshape(bsz, seq, N_KV_HEADS, HEAD_DIM)
        v = proj[..., ATTN_WIDTH + KV_WIDTH:ATTN_WIDTH + 2 * KV_WIDTH].reshape(
            bsz, seq, N_KV_HEADS, HEAD_DIM)
        u = proj[..., ATTN_WIDTH + 2 * KV_WIDTH:]
        q = _rms(q, q_norm_g[l])
        k = _rms(k, k_norm_g[l])
        attn = _swa_sink_attention(q, k, v.astype(jnp.float32), sinks[l])
        ssm = _s5_mixer(u, lam_re[l], lam_im[l], log_dt[l], b_re[l], b_im[l],
                        c_re[l], c_im[l], d_skip[l], w_glu[l])
        mix = jnp.concatenate([_rms(attn, attn_out_g[l]), _rms(ssm, ssm_out_g[l])],
                              axis=-1).astype(x.dtype)
        x = x + mix @ w_o[l]
        hn = _rms(x, ffn_norm_g[l]).astype(x.dtype)
        if l % 2 == 0:
            i = l // 2
            x = x + _swiglu(hn, dense_wg[i], dense_wu[i], dense_wd[i])
        else:
            i = l // 2
            y = _moe_swiglu(hn.reshape(bsz * seq, D_MODEL), router_w[i],
                            moe_wg[i], moe_wu[i], moe_wd[i])
            x = x + y.reshape(bsz, seq, D_MODEL)
    return x
```

```python
from contextlib import ExitStack
import numpy as np
import concourse.bass as bass
import concourse.mybir as mybir

F32 = mybir.dt.float32
BF16 = mybir.dt.bfloat16
I32 = mybir.dt.int32
ALU = mybir.AluOpType
AF = mybir.ActivationFunctionType
AX = mybir.AxisListType

ENGINES = ("pe", "act", "dve", "pool", "sp")


class Buf:
    __slots__ = ("name", "w", "readers", "grp")

    def __init__(self, name, grp=None):
        self.name = name
        self.w = None
        self.readers = []
        self.grp = grp


class Op:
    __slots__ = ("eng", "fn", "deps", "signal", "seq", "is_dma", "grp", "dval", "idx")

    def __init__(self, eng, fn):
        self.eng = eng
        self.fn = fn
        self.deps = []
        self.signal = False
        self.seq = 0
        self.is_dma = False
        self.grp = None
        self.dval = 0
        self.idx = 0


class KB:
    def __init__(self, nc):
        self.nc = nc
        self.ops = {e: [] for e in ENGINES}
        self.n = 0
        self.grp_count = {}
        self.grp_unit = {}
        self.stack = ExitStack()
        self.uid = 0

    def sb(self, shape, dtype, name=None):
        self.uid += 1
        name = name or f"sb{self.uid}"
        return self.stack.enter_context(self.nc.sbuf_tensor(name, list(shape), dtype))

    def ps(self, shape, dtype=F32, name=None):
        self.uid += 1
        name = name or f"ps{self.uid}"
        return self.stack.enter_context(self.nc.psum_tensor(name, list(shape), dtype))

    def buf(self, name, grp=None):
        return Buf(name, grp)

    def _track(self, op, reads, writes):
        deps = []
        for r in reads:
            if r.w is not None:
                deps.append((r.w, "raw"))
        for w in writes:
            if w.w is not None:
                deps.append((w.w, "waw"))
            for rd in w.readers:
                deps.append((rd, "war"))
        seen = set()
        for d, kind in deps:
            if d is op or id(d) in seen:
                continue
            if (not d.is_dma) and d.eng == op.eng and not op.is_dma:
                if kind != "raw" or op.eng == "pe":
                    continue
            seen.add(id(d))
            op.deps.append(d)
        for r in reads:
            if not op.is_dma:
                r.readers = [x for x in r.readers if x.is_dma or x.eng != op.eng]
            r.readers.append(op)
        for w in writes:
            w.w = op
            w.readers = []

    def op(self, eng, fn, reads=(), writes=()):
        o = Op(eng, fn)
        o.idx = self.n
        self.n += 1
        self._track(o, reads, writes)
        self.ops[eng].append(o)
        return o

    def dma(self, eng, out, in_, reads=(), writes=(), **kw):
        assert len(writes) >= 1
        grp = writes[0].grp or writes[0].name
        o = Op(eng, lambda e: e.dma_start(out=out, in_=in_, **kw))
        o.is_dma = True
        o.grp = grp
        o.idx = self.n
        self.n += 1
        self.grp_count[grp] = self.grp_count.get(grp, 0) + 1
        self._track(o, reads, writes)
        self.ops[eng].append(o)
        return o

    def custom_dma_like(self, eng, fn, grp, reads=(), writes=(), unit=1):
        o = Op(eng, fn)
        o.is_dma = True
        o.grp = grp
        o.idx = self.n
        self.n += 1
        self.grp_count[grp] = self.grp_count.get(grp, 0) + 1
        self.grp_unit[grp] = unit
        self._track(o, reads, writes)
        self.ops[eng].append(o)
        return o

    def allgather(self, cc_in, cc_out, dst_sb, dst_view_of_out, reads, writes, ncores=8):
        self.cc_n = getattr(self, "cc_n", 0) + 1
        k = self.cc_n

        def fn(e):
            e.collective_compute("AllGather", ALU.bypass, replica_groups=[list(range(ncores))],
                                 ins=[cc_in.opt()], outs=[cc_out.opt()]).then_inc(self.cc_sem, 1)
            e.wait_ge(self.cc_sem, k)
            return e.dma_start(out=dst_sb, in_=dst_view_of_out)
        assert len(writes) >= 1
        grp = writes[0].grp or writes[0].name
        o = Op("pool", fn)
        o.is_dma = True
        o.grp = grp
        o.idx = self.n
        self.n += 1
        self.grp_count[grp] = self.grp_count.get(grp, 0) + 1
        self._track(o, reads, writes)
        self.ops["pool"].append(o)
        return o

    def emit(self, final_groups=()):
        nc = self.nc
        st = self.stack
        all_ops = sorted([o for e in ENGINES for o in self.ops[e]], key=lambda o: o.idx)
        running = {}
        grp_at = {}
        for o in all_ops:
            grp_at[o.idx] = dict(running) if False else None
        running = {}
        dma_wait_val = {}
        for o in all_ops:
            for d in o.deps:
                if d.is_dma:
                    dma_wait_val[(o.idx, d.grp)] = self.grp_unit.get(d.grp, 16) * running.get(d.grp, 0)
            if o.is_dma:
                running[o.grp] = running.get(o.grp, 0) + 1
        for o in all_ops:
            for d in o.deps:
                if not d.is_dma:
                    d.signal = True
        for e in ENGINES:
            c = 0
            for o in self.ops[e]:
                if o.signal and not o.is_dma:
                    c += 1
                    o.seq = c
        esem = {e: st.enter_context(nc.semaphore(f"s_{e}")) for e in ENGINES}
        gsem = {g: st.enter_context(nc.semaphore(f"g_{g}")) for g in self.grp_count}
        self.cc_sem = st.enter_context(nc.semaphore("cc_sem"))
        self.nsem = len(esem) + len(gsem) + 1
        block = st.enter_context(nc.Block())
        final_total = {g: self.grp_unit.get(g, 16) * self.grp_count[g] for g in final_groups}

        def run(ename, eng):
            waited = {}
            for o in self.ops[ename]:
                for d in o.deps:
                    if d.is_dma:
                        key = ("g", d.grp)
                        sem = gsem[d.grp]
                        val = dma_wait_val[(o.idx, d.grp)]
                    else:
                        key = ("e", d.eng)
                        sem = esem[d.eng]
                        val = d.seq
                    if waited.get(key, 0) >= val:
                        continue
                    waited[key] = val
                    eng.wait_ge(sem, val)
                ins = o.fn(eng)
                if o.is_dma:
                    ins.then_inc(gsem[o.grp], self.grp_unit.get(o.grp, 16))
                elif o.signal:
                    ins.then_inc(esem[ename], 1)
            if ename == "sp":
                for g, v in final_total.items():
                    eng.wait_ge(gsem[g], v)

        @block.sync
        def _(e):
            run("sp", e)

        @block.scalar
        def _(e):
            run("act", e)

        @block.vector
        def _(e):
            run("dve", e)

        @block.gpsimd
        def _(e):
            run("pool", e)

        @block.tensor
        def _(e):
            run("pe", e)

    def close(self):
        self.stack.close()

import math

NTOK = 2048
NPRE = 6144
TT = 512
DM = 1024
EPS = 1e-6
WIN_COLS = 1408
QOFF, KOFF, VOFF, UOFF = 0, 512, 768, 896
TCH = 8
NCH = NTOK // TCH
TWO_PI = 2.0 * math.pi


class PsumPool:
    def __init__(self, kb, n=8):
        self.t = [kb.ps([128, 512], F32, name=f"psb{i}") for i in range(n)]
        self.b = [kb.buf(f"psb{i}") for i in range(n)]
        self.i = 0

    def get(self):
        i = self.i
        self.i = (self.i + 1) % len(self.t)
        return self.t[i], self.b[i]


def make_consts(kb, full=True):
    c = {}
    B = kb.buf
    ones_f = kb.sb([128, 256], F32, name="ones_f"); b_ones_f = B("ones_f")
    kb.op("pool", lambda e: e.memset(ones_f[:], 1.0), writes=[b_ones_f])
    ones_bf = kb.sb([128, 128], BF16, name="ones_bf"); b_ones_bf = B("ones_bf")
    kb.op("pool", lambda e: e.memset(ones_bf[:], 1.0), writes=[b_ones_bf])
    ident = kb.sb([128, 128], BF16, name="ident"); b_ident = B("ident")
    kb.op("pool", lambda e: e.affine_select(out=ident[:], in_=ones_f[:, 0:128], pattern=[[1, 128]], compare_op=ALU.is_equal,
                                            fill=0.0, base=0, channel_multiplier=-1), reads=[b_ones_f], writes=[b_ident])
    epst = kb.sb([128, 1], F32, name="epst"); b_eps = B("epst")
    kb.op("pool", lambda e: e.memset(epst[:], EPS), writes=[b_eps])
    c["eps"] = (epst, b_eps)
    c.update(ones_f=(ones_f, b_ones_f), ones_bf=(ones_bf, b_ones_bf), ident=(ident, b_ident))
    if not full:
        return c
    zeros_f = kb.sb([128, 256], F32, name="zeros_f"); b_zeros_f = B("zeros_f")
    kb.op("pool", lambda e: e.memset(zeros_f[:], 0.0), writes=[b_zeros_f])
    c["zeros_f"] = (zeros_f, b_zeros_f)
    blk64 = kb.sb([128, 128], BF16, name="blk64"); b_blk64 = B("blk64")
    kb.op("pool", lambda e: e.memset(blk64[:], 0.0), writes=[b_blk64])
    kb.op("pool", lambda e: e.memset(blk64[0:64, 0:64], 1.0), writes=[b_blk64])
    kb.op("pool", lambda e: e.memset(blk64[64:128, 64:128], 1.0), writes=[b_blk64])
    bd_tmp = kb.sb([128, 128], F32, name="bd_tmp"); b_bd_tmp = B("bd_tmp")
    bdmask = kb.sb([128, 128], F32, name="bdmask"); b_bdmask = B("bdmask")
    kb.op("pool", lambda e: e.affine_select(out=bd_tmp[:], in_=ones_f[:, 0:128], pattern=[[-16, 8], [0, 16]], compare_op=ALU.is_ge,
                                            fill=0.0, base=0, channel_multiplier=1), reads=[b_ones_f], writes=[b_bd_tmp])
    kb.op("pool", lambda e: e.affine_select(out=bdmask[:], in_=bd_tmp[:], pattern=[[16, 8], [0, 16]], compare_op=ALU.is_ge,
                                            fill=0.0, base=15, channel_multiplier=-1), reads=[b_bd_tmp], writes=[b_bdmask])
    maskneg = kb.sb([128, 256], BF16, name="maskneg"); b_maskneg = B("maskneg")
    kb.op("pool", lambda e: e.affine_select(out=maskneg[:, 0:128], in_=zeros_f[:, 0:128], pattern=[[1, 128]], compare_op=ALU.is_ge,
                                            fill=-30000.0, base=0, channel_multiplier=-1), reads=[b_zeros_f], writes=[b_maskneg])
    kb.op("pool", lambda e: e.affine_select(out=maskneg[:, 128:256], in_=zeros_f[:, 0:128], pattern=[[-1, 128]], compare_op=ALU.is_ge,
                                            fill=-30000.0, base=-1, channel_multiplier=1), reads=[b_zeros_f], writes=[b_maskneg])
    c.update(blk64=(blk64, b_blk64), bdmask=(bdmask, b_bdmask), maskneg=(maskneg, b_maskneg))
    return c


def rms_tile(kb, pp, C, x_ap, bx, g_ap, bg, out_ap, bout, nchunks, ntok, scratch, bscr, rstd, brstd, dim):
    ones_bf, b_ones = C["ones_bf"]
    ps, bps = pp.get()
    for c in range(nchunks):
        kb.op("act", lambda e, c=c: e.activation(out=scratch[:, c, 0:ntok], in_=x_ap[:, c, :], func=AF.Square),
              reads=[bx], writes=[bscr])
    for c in range(nchunks):
        kb.op("pe", lambda e, c=c: e.matmul(ps[:, 0:ntok], lhsT=ones_bf[:], rhs=scratch[:, c, 0:ntok], start=(c == 0), stop=(c == nchunks - 1)),
              reads=[b_ones, bscr], writes=[bps])
    epst, beps = C["eps"]
    kb.op("act", lambda e: e.activation(out=rstd[:, 0:ntok], in_=ps[:, 0:ntok], func=AF.Sqrt, scale=1.0 / dim, bias=epst[:, 0:1]),
          reads=[bps, beps], writes=[brstd])
    kb.op("dve", lambda e: e.reciprocal(out=rstd[:, 0:ntok], in_=rstd[:, 0:ntok]), reads=[brstd], writes=[brstd])
    for c in range(nchunks):
        eng = "dve"
        kb.op(eng, lambda e, c=c: e.scalar_tensor_tensor(out=out_ap[:, c, :], in0=x_ap[:, c, :], scalar=g_ap[:, c:c + 1], in1=rstd[:, 0:ntok],
                                                         op0=ALU.mult, op1=ALU.mult),
              reads=[bx, bg, brstd], writes=[bout])


DEBUG = False


def build_mixer():
    nc = bass.Bass("TRN2", target_bir_lowering=False)

    def D(name, shape, dt=F32, kind="ExternalInput"):
        return nc.dram_tensor(name, list(shape), dt, kind=kind).ap()
    xT_all = D("xT_all", [128, 8, NPRE + NTOK])
    flag_d = D("flag", [128, 1]); g1_d = D("g1", [128, 8])
    w_in_d = D("w_in", [DM, WIN_COLS])
    qg_d = D("qg", [128, 1]); kg_d = D("kg", [128, 1]); snk_d = D("snk", [128, 4])
    lamre_d = D("lamre", [128, 16]); lamim_d = D("lamim", [128, 16]); ldt_d = D("ldt", [128, 16])
    bre_d = D("bre", [128, 16, 16]); bim_d = D("bim", [128, 16, 16]); cre_d = D("cre", [128, 16, 16]); cim_d = D("cim", [128, 16, 16])
    dsk_d = D("dsk", [128, 4]); w_glu_d = D("w_glu", [512, 512]); ag_d = D("ag", [128, 4]); sg_d = D("sg", [128, 4])
    w_o_d = D("w_o", [DM, DM])
    xT_out = D("xT_out", [128, 8, NTOK], kind="ExternalOutput")
    upre = nc.dram_tensor("upre", [128, 4, NPRE + NTOK], BF16, kind="Internal").ap()
    if DEBUG:
        dbg_ssm = D("dbg_ssm", [128, 4, NTOK], BF16, kind="ExternalOutput")
        dbg_attn = D("dbg_attn", [128, 4, NTOK], BF16, kind="ExternalOutput")
        dbg_yg = D("dbg_yg", [128, 4, NTOK], BF16, kind="ExternalOutput")
        dbg_q = D("dbg_q", [128, 4, NTOK], BF16, kind="ExternalOutput")
        dbg_k = D("dbg_k", [128, 2, 128 + NTOK], BF16, kind="ExternalOutput")
        dbg_v = D("dbg_v", [128, 17, 128], BF16, kind="ExternalOutput")

    kb = KB(nc)
    B = kb.buf
    C = make_consts(kb)
    pp = PsumPool(kb, 4)
    eps_t = kb.ps([128, 2, 4, NCH], F32, name="eps_t"); b_eps_t = B("eps_t")
    dram_in = B("dram_in")
    b_out = B("xT_out", grp="out")
    b_upre = [B(f"upre{s}") for s in range(4)]

    def load_small(name, src, shape):
        t = kb.sb(shape, F32, name=name); b = B(name, grp="const")
        kb.dma("sp", t[:], src, reads=[dram_in], writes=[b])
        return t, b
    flag, b_flag = load_small("flag_s", flag_d, [128, 1])
    g1, b_g1 = load_small("g1_s", g1_d, [128, 8])
    qg, b_qg = load_small("qg_s", qg_d, [128, 1]); kg, b_kg = load_small("kg_s", kg_d, [128, 1])
    snk, b_snk = load_small("snk_s", snk_d, [128, 4])
    lamre, b_lamre = load_small("lamre_s", lamre_d, [128, 16]); lamim, b_lamim = load_small("lamim_s", lamim_d, [128, 16])
    ldt, b_ldt = load_small("ldt_s", ldt_d, [128, 16])
    bre, b_bre = load_small("bre_s", bre_d, [128, 16, 16]); bim, b_bim = load_small("bim_s", bim_d, [128, 16, 16])
    cre, b_cre = load_small("cre_s", cre_d, [128, 16, 16]); cim, b_cim = load_small("cim_s", cim_d, [128, 16, 16])
    dsk, b_dsk = load_small("dsk_s", dsk_d, [128, 4]); ag, b_ag = load_small("ag_s", ag_d, [128, 4]); sg, b_sg = load_small("sg_s", sg_d, [128, 4])

    wreg = kb.sb([128, 8 * WIN_COLS], BF16, name="wreg"); b_wreg = B("wreg")
    w_in = wreg[:, :].rearrange("p (c n) -> p c n", n=WIN_COLS)
    w_o = wreg[:, 0:8 * DM].rearrange("p (c n) -> p c n", n=DM)
    w_glu = wreg[:, 8 * DM:8 * DM + 4 * 512].rearrange("p (c n) -> p c n", n=512)
    w_in_v = w_in_d.rearrange("(c p) n -> p c n", p=128)
    for c in range(8):
        kb.dma("pool", w_in[:, c, :], w_in_v[:, c, :], reads=[dram_in], writes=[b_wreg])

    V = lambda name, shape, dt=F32: (kb.sb(shape, dt, name=name), B(name))
    dt_, b_dt = V("dt_", [128, 16]); xr, b_xr = V("xr", [128, 16]); th, b_th = V("th", [128, 16])
    kb.op("act", lambda e: e.activation(out=dt_[:], in_=ldt[:], func=AF.Exp), reads=[b_ldt], writes=[b_dt])
    kb.op("dve", lambda e: e.tensor_tensor(out=xr[:], in0=lamre[:], in1=dt_[:], op=ALU.mult), reads=[b_lamre, b_dt], writes=[b_xr])
    kb.op("dve", lambda e: e.tensor_tensor(out=th[:], in0=lamim[:], in1=dt_[:], op=ALU.mult), reads=[b_lamim, b_dt], writes=[b_th])
    mvals, b_mvals = V("mvals", [128, 9, 16])
    for m in range(9):
        kb.op("pool", lambda e, m=m: e.memset(mvals[:, m, :], float(m)), writes=[b_mvals])
    ang, b_ang = V("ang", [128, 2, 9, 16]); mag, b_mag = V("mag", [128, 9, 16])
    kb.op("dve", lambda e: e.tensor_tensor(out=ang[:, 0], in0=mvals[:], in1=th[:].unsqueeze(1).to_broadcast([128, 9, 16]), op=ALU.mult),
          reads=[b_mvals, b_th], writes=[b_ang])
    kb.op("dve", lambda e: e.tensor_scalar(out=ang[:, 1], in0=ang[:, 0], scalar1=math.pi / 2, scalar2=None, op0=ALU.add), reads=[b_ang], writes=[b_ang])
    kb.op("dve", lambda e: e.tensor_tensor(out=mag[:], in0=mvals[:], in1=xr[:].unsqueeze(1).to_broadcast([128, 9, 16]), op=ALU.mult),
          reads=[b_mvals, b_xr], writes=[b_mag])
    kb.op("act", lambda e: e.activation(out=mag[:], in_=mag[:], func=AF.Exp), reads=[b_mag], writes=[b_mag])

    def range_reduce(x_ap, bx, n, tf, b_tf, ti, b_ti):
        kb.op("dve", lambda e: e.tensor_scalar(out=tf, in0=x_ap, scalar1=1.0 / TWO_PI, scalar2=None, op0=ALU.mult), reads=[bx], writes=[b_tf])
        kb.op("dve", lambda e: e.tensor_copy(out=ti, in_=tf), reads=[b_tf], writes=[b_ti])
        kb.op("dve", lambda e: e.tensor_copy(out=tf, in_=ti), reads=[b_ti], writes=[b_tf])
        kb.op("dve", lambda e: e.scalar_tensor_tensor(out=x_ap, in0=tf, scalar=-TWO_PI, in1=x_ap, op0=ALU.mult, op1=ALU.add),
              reads=[b_tf, bx], writes=[bx])
    angf = ang[:].rearrange("p a m n -> p (a m n)")
    rr_tf, b_rr_tf = V("rr_tf", [128, 288]); rr_ti, b_rr_ti = V("rr_ti", [128, 288], I32)
    range_reduce(angf, b_ang, 2 * 9 * 16, rr_tf[:], b_rr_tf, rr_ti[:], b_rr_ti)
    sc, b_sc = V("sc", [128, 2, 9, 16])
    kb.op("act", lambda e: e.activation(out=sc[:].rearrange("p a m n -> p (a m n)"), in_=angf, func=AF.Sin), reads=[b_ang], writes=[b_sc])
    apr, b_apr = V("apr", [128, 9, 16]); api, b_api = V("api", [128, 9, 16])
    kb.op("dve", lambda e: e.tensor_tensor(out=apr[:], in0=mag[:], in1=sc[:, 1], op=ALU.mult), reads=[b_mag, b_sc], writes=[b_apr])
    kb.op("dve", lambda e: e.tensor_tensor(out=api[:], in0=mag[:], in1=sc[:, 0], op=ALU.mult), reads=[b_mag, b_sc], writes=[b_api])
    nr, b_nr = V("nr", [128, 16]); den, b_den = V("den", [128, 16]); t1, b_t1 = V("t1", [128, 16]); t2, b_t2 = V("t2", [128, 16])
    fre, b_fre = V("fre", [128, 16]); fim, b_fim = V("fim", [128, 16])
    kb.op("dve", lambda e: e.tensor_scalar(out=nr[:], in0=apr[:, 1, :], scalar1=-1.0, scalar2=None, op0=ALU.add), reads=[b_apr], writes=[b_nr])
    kb.op("dve", lambda e: e.tensor_tensor(out=t1[:], in0=lamre[:], in1=lamre[:], op=ALU.mult), reads=[b_lamre], writes=[b_t1])
    kb.op("dve", lambda e: e.tensor_tensor(out=t2[:], in0=lamim[:], in1=lamim[:], op=ALU.mult), reads=[b_lamim], writes=[b_t2])
    kb.op("dve", lambda e: e.tensor_tensor(out=den[:], in0=t1[:], in1=t2[:], op=ALU.add), reads=[b_t1, b_t2], writes=[b_den])
    kb.op("dve", lambda e: e.reciprocal(out=den[:], in_=den[:]), reads=[b_den], writes=[b_den])
    kb.op("dve", lambda e: e.tensor_tensor(out=t1[:], in0=nr[:], in1=lamre[:], op=ALU.mult), reads=[b_nr, b_lamre, b_den], writes=[b_t1])
    kb.op("dve", lambda e: e.tensor_tensor(out=t2[:], in0=api[:, 1, :], in1=lamim[:], op=ALU.mult), reads=[b_api, b_lamim, b_den], writes=[b_t2])
    kb.op("dve", lambda e: e.tensor_tensor(out=fre[:], in0=t1[:], in1=t2[:], op=ALU.add), reads=[b_t1, b_t2], writes=[b_fre])
    kb.op("dve", lambda e: e.tensor_tensor(out=fre[:], in0=fre[:], in1=den[:], op=ALU.mult), reads=[b_fre, b_den], writes=[b_fre])
    kb.op("dve", lambda e: e.tensor_tensor(out=t1[:], in0=api[:, 1, :], in1=lamre[:], op=ALU.mult), reads=[b_api, b_lamre, b_fre], writes=[b_t1])
    kb.op("dve", lambda e: e.tensor_tensor(out=t2[:], in0=nr[:], in1=lamim[:], op=ALU.mult), reads=[b_nr, b_lamim, b_fre], writes=[b_t2])
    kb.op("dve", lambda e: e.tensor_tensor(out=fim[:], in0=t1[:], in1=t2[:], op=ALU.subtract), reads=[b_t1, b_t2], writes=[b_fim])
    kb.op("dve", lambda e: e.tensor_tensor(out=fim[:], in0=fim[:], in1=den[:], op=ALU.mult), reads=[b_fim, b_den], writes=[b_fim])
    bbr, b_bbr = V("bbr", [128, 16, 16]); bbi, b_bbi = V("bbi", [128, 16, 16]); t3, b_t3 = V("t3", [128, 16, 16])
    fre_b = fre[:].unsqueeze(2).to_broadcast([128, 16, 16]); fim_b = fim[:].unsqueeze(2).to_broadcast([128, 16, 16])
    kb.op("dve", lambda e: e.tensor_tensor(out=bbr[:], in0=bre[:], in1=fre_b, op=ALU.mult), reads=[b_bre, b_fre], writes=[b_bbr])
    kb.op("dve", lambda e: e.tensor_tensor(out=t3[:], in0=bim[:], in1=fim_b, op=ALU.mult), reads=[b_bim, b_fim], writes=[b_t3])
    kb.op("dve", lambda e: e.tensor_tensor(out=bbr[:], in0=bbr[:], in1=t3[:], op=ALU.subtract), reads=[b_bbr, b_t3], writes=[b_bbr])
    kb.op("dve", lambda e: e.tensor_tensor(out=bbi[:], in0=bim[:], in1=fre_b, op=ALU.mult), reads=[b_bim, b_fre, b_t3], writes=[b_bbi])
    kb.op("dve", lambda e: e.tensor_tensor(out=t3[:], in0=bre[:], in1=fim_b, op=ALU.mult), reads=[b_bre, b_fim, b_bbr], writes=[b_t3])
    kb.op("dve", lambda e: e.tensor_tensor(out=bbi[:], in0=bbi[:], in1=t3[:], op=ALU.add), reads=[b_bbi, b_t3], writes=[b_bbi])
    esnk, b_esnk = V("esnk", [128, 4])
    kb.op("act", lambda e: e.activation(out=esnk[:], in_=snk[:], func=AF.Exp), reads=[b_snk], writes=[b_esnk])
    kv_i, b_kvi = V("kv_i", [128, NCH + 1], I32); kvals, b_kvals = V("kvals", [128, NCH + 1])
    kb.op("pool", lambda e: e.iota(kv_i[:], pattern=[[1, NCH + 1]], base=0, channel_multiplier=0), writes=[b_kvi])
    kb.op("dve", lambda e: e.tensor_copy(out=kvals[:], in_=kv_i[:]), reads=[b_kvi], writes=[b_kvals])


    ones_bf, b_ones_bf = C["ones_bf"]; ident, b_ident = C["ident"]; blk64, b_blk64 = C["blk64"]
    bdmask, b_bdmask = C["bdmask"]; maskneg, b_maskneg = C["maskneg"]; epst, b_epst = C["eps"]
    bdmask3 = bdmask[:].rearrange("p (g c) -> p g c", c=16)

    qT, b_qT = V("qT", [128, 4, NTOK], BF16)
    ygT, b_ygT = V("ygT", [128, 4, NTOK], BF16)
    kT, b_kT = V("kT", [128, 2, 128 + NTOK], BF16)
    vtm, b_vtm = V("vtm", [128, 17, 128], BF16)
    onesfl, b_onesfl = V("onesfl", [128, 64], BF16)
    kb.op("dve", lambda e: e.tensor_scalar(out=onesfl[:], in0=C["ones_f"][0][:, 0:64], scalar1=flag[:, 0:1], scalar2=None, op0=ALU.mult),
          reads=[C["ones_f"][1], b_flag], writes=[b_onesfl])

    xraw, b_xraw = V("xraw", [128, 4112])
    xt = [(xraw[:, 0:4096].rearrange("p (c t) -> p c t", t=TT), b_xraw)]
    hraw = [V(f"hraw{i}", [128, 2080]) for i in range(2)]
    ht = [(hraw[i][0].bitcast(BF16)[:, 0:8 * TT].rearrange("p (c t) -> p c t", t=TT), hraw[i][1]) for i in range(2)]
    sq, b_sq = V("sq", [128, 8, TT], BF16)
    rstd, b_rstd = V("rstd", [128, TT])
    ustage = [V(f"ustage{i}", [128, 4, TT], BF16) for i in range(2)]

    def norm_tile(tok0, idx):
        x_t, b_x = xt[0]
        h_t, b_h = ht[idx % 2]
        kb.dma("sp", x_t[:], xT_all[:, :, tok0:tok0 + TT], reads=[dram_in], writes=[b_x])
        rms_tile(kb, pp, C, x_t, b_x, g1, b_g1, h_t, b_h, 8, TT, sq, b_sq, rstd, b_rstd, DM)
        return h_t, b_h

    def proj(h_t, b_h, col0, ntile, consume, ncols=TT, tok_sl=slice(0, TT)):
        for m in range(ntile):
            ps, bps = pp.get()
            for c in range(8):
                kb.op("pe", lambda e, m=m, c=c, ps=ps: e.matmul(ps[:, 0:ncols], lhsT=w_in[:, c, col0 + 128 * m: col0 + 128 * (m + 1)],
                                                                rhs=h_t[:, c, tok_sl], start=(c == 0), stop=(c == 7)),
                      reads=[b_wreg, b_h], writes=[bps])
            consume(m, ps, bps)

    qkbuf = [(V(f"sqh{i}", [128, TT], BF16), V(f"r2_{i}", [128, TT])) for i in range(2)]
    qkn = [0]

    def qk_norm(ps, bps, ncols, gain, b_gain, out_ap, b_out_):
        sqh, b_sqh = qkbuf[qkn[0] % 2][0]
        kb.op("act", lambda e: e.activation(out=sqh[:, 0:ncols], in_=ps[:, 0:ncols], func=AF.Square), reads=[bps], writes=[b_sqh])
        ps2, bps2 = pp.get()
        kb.op("pe", lambda e: e.matmul(ps2[:, 0:ncols], lhsT=blk64[:], rhs=sqh[:, 0:ncols], start=True, stop=True),
              reads=[b_blk64, b_sqh], writes=[bps2])
        r2, b_r2 = qkbuf[qkn[0] % 2][1]
        qkn[0] += 1
        kb.op("act", lambda e: e.activation(out=r2[:, 0:ncols], in_=ps2[:, 0:ncols], func=AF.Sqrt, scale=1.0 / 64, bias=epst[:, 0:1]),
              reads=[bps2, b_epst], writes=[b_r2])
        kb.op("dve", lambda e: e.reciprocal(out=r2[:, 0:ncols], in_=r2[:, 0:ncols]), reads=[b_r2], writes=[b_r2])
        kb.op("dve", lambda e: e.scalar_tensor_tensor(out=out_ap, in0=ps[:, 0:ncols], scalar=gain[:, 0:1], in1=r2[:, 0:ncols], op0=ALU.mult, op1=ALU.mult),
              reads=[bps, b_gain, b_r2], writes=[b_out_])

    nt = 0
    for seg in range(3):
        for tt in range(4):
            tok0 = seg * NTOK + tt * TT
            h_t, b_h = norm_tile(tok0, nt)
            us, b_us = ustage[nt % 2]
            nt += 1

            def cons_u(m, ps, bps, us=us, b_us=b_us):
                kb.op("act", lambda e: e.activation(out=us[:, m, :], in_=ps[:, :], func=AF.Copy), reads=[bps], writes=[b_us])
            proj(h_t, b_h, UOFF, 4, cons_u)
            kb.dma("sp", upre[:, :, tok0:tok0 + TT], us[:], reads=[b_us], writes=[b_upre[seg]])
            if seg == 2 and tt == 3:
                hsl = slice(TT - 128, TT)

                def cons_kh(m, ps, bps):
                    qk_norm(ps, bps, 128, kg, b_kg, kT[:, m, 0:128], b_kT)
                proj(h_t, b_h, KOFF, 2, cons_kh, ncols=128, tok_sl=hsl)
                ps, bps = pp.get()
                for c in range(8):
                    kb.op("pe", lambda e, c=c, ps=ps, h_t=h_t, hsl=hsl: e.matmul(ps[:, 0:128], lhsT=h_t[:, c, hsl], rhs=w_in[:, c, VOFF:VOFF + 128], start=(c == 0), stop=(c == 7)),
                          reads=[b_wreg, b_h], writes=[bps])
                kb.op("dve", lambda e, ps=ps: e.tensor_scalar(out=vtm[:, 0, :], in0=ps[:, 0:128], scalar1=flag[:, 0:1], scalar2=None, op0=ALU.mult),
                      reads=[bps, b_flag], writes=[b_vtm])

    for tt in range(4):
        tok0 = NPRE + tt * TT
        o0 = tt * TT
        h_t, b_h = norm_tile(tok0, nt)
        nt += 1

        def cons_q(m, ps, bps, o0=o0):
            qk_norm(ps, bps, TT, qg, b_qg, qT[:, m, o0:o0 + TT], b_qT)
        proj(h_t, b_h, QOFF, 4, cons_q)

        def cons_k(m, ps, bps, o0=o0):
            qk_norm(ps, bps, TT, kg, b_kg, kT[:, m, 128 + o0:128 + o0 + TT], b_kT)
        proj(h_t, b_h, KOFF, 2, cons_k)

        us, b_us = ustage[nt % 2]

        def cons_u2(m, ps, bps, us=us, b_us=b_us):
            kb.op("act", lambda e: e.activation(out=us[:, m, :], in_=ps[:, :], func=AF.Copy), reads=[bps], writes=[b_us])
        proj(h_t, b_h, UOFF, 4, cons_u2)
        kb.dma("sp", upre[:, :, tok0:tok0 + TT], us[:], reads=[b_us], writes=[b_upre[3]])
        for bl in range(4):
            ps, bps = pp.get()
            for c in range(8):
                kb.op("pe", lambda e, c=c, ps=ps, bl=bl, h_t=h_t: e.matmul(ps[:, 0:128], lhsT=h_t[:, c, bl * 128:(bl + 1) * 128], rhs=w_in[:, c, VOFF:VOFF + 128],
                                                                 start=(c == 0), stop=(c == 7)),
                      reads=[b_wreg, b_h], writes=[bps])
            kb.op("act", lambda e, ps=ps, bl=bl, tt=tt: e.activation(out=vtm[:, 1 + tt * 4 + bl, :], in_=ps[:, 0:128], func=AF.Copy), reads=[bps], writes=[b_vtm])

    w_o_v = w_o_d.rearrange("(c p) n -> p c n", p=128)
    for c in range(8):
        kb.dma("pool", w_o[:, c, :], w_o_v[:, c, :], reads=[dram_in], writes=[b_wreg])
    kb.dma("pool", w_glu, w_glu_d.rearrange("(c p) n -> p c n", p=128), reads=[dram_in], writes=[b_wreg])

    Pre, b_Pre = V("Pre", [128, 8, 4, 16]); Pim, b_Pim = V("Pim", [128, 8, 4, 16]); Ptmp, b_Ptmp = V("Ptmp", [128, 8, 4, 16])
    cimn, b_cimn = V("cimn", [128, 16, 16])
    kb.op("dve", lambda e: e.tensor_scalar(out=cimn[:], in0=cim[:], scalar1=-1.0, scalar2=None, op0=ALU.mult), reads=[b_cim], writes=[b_cimn])
    Wst, b_Wst = V("Wst", [128, 8, 8, 128], BF16)
    Wout, b_Wout = V("Wout", [128, 8, 8, 128], BF16)
    Kfir, b_Kfir = V("Kfir", [128, 8, 128], BF16)
    BDC, b_BDC = V("BDC", [128, 8, 128], BF16)
    bdP = [V(f"bdP{i}", [128, 8, 128], BF16) for i in range(2)]
    tab = hraw[1][0][:, 0:2 * 4 * (NCH + 1)].rearrange("p (a n k) -> p a n k", a=2, n=4); b_tab = hraw[1][1]
    tabf = hraw[1][0][:, 0:2 * 4 * (NCH + 1)]
    xflat = xraw[:, :]
    xt_i32 = xraw.bitcast(I32)[:, :]
    Mv = lambda i: xflat[:, i * 1024:(i + 1) * 1024].rearrange("p (n k) -> p n k", n=4)
    Mre, Mim, Mt1, Mt2 = Mv(0), Mv(1), Mv(2), Mv(3)
    b_Mre = b_Mim = b_Mt1 = b_Mt2 = xt[0][1]
    Wx = hraw[0][0][:, 0:2 * 4 * (NCH + 1)].rearrange("p (a n k) -> p a n k", a=2, n=4); b_Wx = hraw[0][1]
    Sprev, b_Sprev = V("Sprev", [128, 2, 4, NCH], BF16)
    useg = [(ustage[i][0][:].rearrange("p c t -> p (c t)"), ustage[i][1]) for i in range(2)]
    ident_f, b_ident_f = V("ident_f", [128, 128])
    kb.op("dve", lambda e: e.tensor_copy(out=ident_f[:], in_=ident[:]), reads=[b_ident], writes=[b_ident_f])
    nseg = 0
    for t4 in range(4):
        tsl = slice(t4 * 4, t4 * 4 + 4)
        a_r = apr[:, 0:8, tsl].unsqueeze(3).to_broadcast([128, 8, 4, 16]); a_i = api[:, 0:8, tsl].unsqueeze(3).to_broadcast([128, 8, 4, 16])
        a1_r = apr[:, 1:9, tsl].unsqueeze(3).to_broadcast([128, 8, 4, 16]); a1_i = api[:, 1:9, tsl].unsqueeze(3).to_broadcast([128, 8, 4, 16])
        bbr_b = bbr[:, tsl, :].unsqueeze(1).to_broadcast([128, 8, 4, 16]); bbi_b = bbi[:, tsl, :].unsqueeze(1).to_broadcast([128, 8, 4, 16])
        cre_b = cre[:, tsl, :].unsqueeze(1).to_broadcast([128, 8, 4, 16]); cim_b = cim[:, tsl, :].unsqueeze(1).to_broadcast([128, 8, 4, 16])
        TTm = lambda o, a, b, op, rd, wr: kb.op("dve", lambda e: e.tensor_tensor(out=o, in0=a, in1=b, op=op), reads=rd, writes=wr)
        TTm(Pre[:], a_r, bbr_b, ALU.mult, [b_apr, b_bbr], [b_Pre]); TTm(Ptmp[:], a_i, bbi_b, ALU.mult, [b_api, b_bbi], [b_Ptmp])
        TTm(Pre[:], Pre[:], Ptmp[:], ALU.subtract, [b_Pre, b_Ptmp], [b_Pre])
        TTm(Pim[:], a_r, bbi_b, ALU.mult, [b_apr, b_bbi], [b_Pim]); TTm(Ptmp[:], a_i, bbr_b, ALU.mult, [b_api, b_bbr, b_Pre], [b_Ptmp])
        TTm(Pim[:], Pim[:], Ptmp[:], ALU.add, [b_Pim, b_Ptmp], [b_Pim])
        def bd_expand(eng, out2d, src16, rd, wr):
            kb.op(eng, lambda e: e.tensor_tensor(out=out2d.rearrange("p (g c) -> p g c", c=16), in0=src16.unsqueeze(1).to_broadcast([128, 8, 16]),
                                                 in1=bdmask3, op=ALU.mult), reads=rd + [b_bdmask], writes=wr)
        for ns in range(4):
            bd_expand("pool", BDC[:, ns * 2 + 0, :], cre[:, t4 * 4 + ns, :], [b_cre], [b_BDC])
            bd_expand("pool", BDC[:, ns * 2 + 1, :], cimn[:, t4 * 4 + ns, :], [b_cimn], [b_BDC])
        for m in range(8):
            bdp, b_bdp = bdP[m % 2]
            for ns in range(4):
                bd_expand("dve", bdp[:, ns * 2 + 0, :], Pre[:, m, ns, :], [b_Pre], [b_bdp])
                bd_expand("dve", bdp[:, ns * 2 + 1, :], Pim[:, m, ns, :], [b_Pim], [b_bdp])
            for half in range(2):
                ps, bps = pp.get()
                for q in range(4):
                    i8 = half * 4 + q
                    kb.op("pe", lambda e, ps=ps, q=q, i8=i8, bdp=bdp: e.matmul(ps[:, q * 128:(q + 1) * 128], lhsT=bdp[:, i8, :], rhs=ident[:], start=True, stop=True),
                          reads=[b_bdp, b_ident], writes=[bps])
                kb.op("act", lambda e, ps=ps, half=half, m=m: e.activation(out=Wst[:, 7 - m, half * 4:(half + 1) * 4, :].rearrange("p a c -> p (a c)"),
                                                                           in_=ps[:, :], func=AF.Copy), reads=[bps], writes=[b_Wst])
            ps, bps = pp.get()
            for i8 in range(8):
                kb.op("pe", lambda e, ps=ps, i8=i8, bdp=bdp: e.matmul(ps[:, 0:128], lhsT=bdp[:, i8, :], rhs=BDC[:, i8, :], start=(i8 == 0), stop=(i8 == 7)),
                      reads=[b_bdp, b_BDC], writes=[bps])
            if m == 0:
                kb.op("dve", lambda e, ps=ps, t4=t4: e.scalar_tensor_tensor(out=Kfir[:, 0, :], in0=ident_f[:], scalar=dsk[:, t4:t4 + 1], in1=ps[:, 0:128],
                                                                           op0=ALU.mult, op1=ALU.add), reads=[bps, b_ident_f, b_dsk], writes=[b_Kfir])
            else:
                kb.op("act", lambda e, ps=ps, m=m: e.activation(out=Kfir[:, m, :], in_=ps[:, 0:128], func=AF.Copy), reads=[bps], writes=[b_Kfir])
        TTm(Pre[:], a1_r, cre_b, ALU.mult, [b_apr, b_cre], [b_Pre]); TTm(Ptmp[:], a1_i, cim_b, ALU.mult, [b_api, b_cim, b_Pim], [b_Ptmp])
        TTm(Pre[:], Pre[:], Ptmp[:], ALU.subtract, [b_Pre, b_Ptmp], [b_Pre])
        TTm(Pim[:], a1_i, cre_b, ALU.mult, [b_api, b_cre], [b_Pim]); TTm(Ptmp[:], a1_r, cim_b, ALU.mult, [b_apr, b_cim, b_Pre], [b_Ptmp])
        TTm(Pim[:], Pim[:], Ptmp[:], ALU.add, [b_Pim, b_Ptmp], [b_Pim])
        kb.op("dve", lambda e: e.tensor_scalar(out=Pim[:], in0=Pim[:], scalar1=-1.0, scalar2=None, op0=ALU.mult), reads=[b_Pim], writes=[b_Pim])

        for j in range(8):
            for ns in range(4):
                bd_expand("pool", Wout[:, j, ns * 2 + 0, :], Pre[:, j, ns, :], [b_Pre], [b_Wout])
                bd_expand("pool", Wout[:, j, ns * 2 + 1, :], Pim[:, j, ns, :], [b_Pim], [b_Wout])
        for ns in range(4):
            ts = t4 * 4 + ns
            kb.op("dve", lambda e, ns=ns, ts=ts: e.tensor_scalar(out=tab[:, 0, ns, :], in0=kvals[:], scalar1=ang[:, 0, 8, ts:ts + 1], scalar2=None, op0=ALU.mult),
                  reads=[b_kvals, b_ang], writes=[b_tab])
        kb.op("dve", lambda e: e.tensor_scalar(out=tab[:, 1], in0=tab[:, 0], scalar1=math.pi / 2, scalar2=None, op0=ALU.add), reads=[b_tab], writes=[b_tab])
        NT_ = 2 * 4 * (NCH + 1)
        range_reduce(tabf, b_tab, NT_, xflat[:, 0:NT_], xt[0][1], xt_i32[:, 2056:2056 + NT_], xt[0][1])
        kb.op("act", lambda e: e.activation(out=tabf, in_=tabf, func=AF.Sin), reads=[b_tab], writes=[b_tab])
        sinM = tab[:, 0, :, 1:NCH + 1]; cosM = tab[:, 1, :, 1:NCH + 1]
        sinD = tab[:, 0, :, 0:NCH]; cosD = tab[:, 1, :, 0:NCH]
        kb.op("pool", lambda e: e.memset(Wx[:, :, :, 0:1], 0.0), writes=[b_Wx])
        for seg in range(4):
            us_, b_us_ = useg[nseg % 2]
            nseg += 1
            kb.dma("sp", us_, upre[:, t4, seg * NTOK:(seg + 1) * NTOK], reads=[b_upre[seg]], writes=[b_us_])
            uv = us_.rearrange("p (k j) -> p j k", j=TCH); b_u = b_us_
            for ns in range(4):
                for ri in range(2):
                    for j in range(8):
                        kb.op("pe", lambda e, ns=ns, ri=ri, j=j, uv=uv: e.matmul(eps_t[:, ri, ns, :], lhsT=Wst[:, j, ns * 2 + ri, :], rhs=uv[:, j, :],
                                                                               start=(j == 0), stop=(j == 7)),
                              reads=[b_Wst, b_u], writes=[b_eps_t])
            TTd = lambda o, a, b, op, rd, wr, eng="dve": kb.op(eng, lambda e: e.tensor_tensor(out=o, in0=a, in1=b, op=op), reads=rd, writes=wr)
            TTd(Mt1[:], eps_t[:, 0], cosM, ALU.mult, [b_eps_t, b_tab], [b_Mt1]); TTd(Mt2[:], eps_t[:, 1], sinM, ALU.mult, [b_eps_t, b_tab], [b_Mt2])
            TTd(Mre[:], Mt1[:], Mt2[:], ALU.add, [b_Mt1, b_Mt2], [b_Mre], "pool")
            TTd(Mt1[:], eps_t[:, 1], cosM, ALU.mult, [b_eps_t, b_tab], [b_Mt1]); TTd(Mt2[:], eps_t[:, 0], sinM, ALU.mult, [b_eps_t, b_tab], [b_Mt2])
            TTd(Mim[:], Mt1[:], Mt2[:], ALU.subtract, [b_Mt1, b_Mt2], [b_Mim], "pool")
            for ns in range(4):
                ts = t4 * 4 + ns
                rho_b = mag[:, 8, ts:ts + 1].to_broadcast([128, NCH])
                kb.op("dve", lambda e, ns=ns, rho_b=rho_b: e.tensor_tensor_scan(out=Wx[:, 0, ns, 1:NCH + 1], data0=rho_b, data1=Mre[:, ns, :],
                                                                              initial=Wx[:, 0, ns, 0:1], op0=ALU.mult, op1=ALU.add),
                      reads=[b_mag, b_Mre, b_Wx], writes=[b_Wx])
                kb.op("dve", lambda e, ns=ns, rho_b=rho_b: e.tensor_tensor_scan(out=Wx[:, 1, ns, 1:NCH + 1], data0=rho_b, data1=Mim[:, ns, :],
                                                                              initial=Wx[:, 1, ns, 0:1], op0=ALU.mult, op1=ALU.add),
                      reads=[b_mag, b_Mim, b_Wx], writes=[b_Wx])
            if seg < 3:
                c256 = tab[:, 1, :, NCH:NCH + 1]; s256 = tab[:, 0, :, NCH:NCH + 1]
                wl_r = Wx[:, 0, :, NCH:NCH + 1]; wl_i = Wx[:, 1, :, NCH:NCH + 1]
                ca, b_ca = V(f"ca{t4}_{seg}", [128, 4, 4, 1])
                TTd(ca[:, 0], wl_r, c256, ALU.mult, [b_Wx, b_tab], [b_ca]); TTd(ca[:, 1], wl_i, s256, ALU.mult, [b_Wx, b_tab], [b_ca])
                TTd(ca[:, 2], wl_r, s256, ALU.mult, [b_Wx, b_tab], [b_ca]); TTd(ca[:, 3], wl_i, c256, ALU.mult, [b_Wx, b_tab], [b_ca])
                TTd(Wx[:, 0, :, 0:1], ca[:, 0], ca[:, 1], ALU.subtract, [b_ca], [b_Wx]); TTd(Wx[:, 1, :, 0:1], ca[:, 2], ca[:, 3], ALU.add, [b_ca], [b_Wx])
            else:
                wr_ = Wx[:, 0, :, 0:NCH]; wi_ = Wx[:, 1, :, 0:NCH]
                TTd(Mt1[:], wr_, cosD, ALU.mult, [b_Wx, b_tab], [b_Mt1]); TTd(Mt2[:], wi_, sinD, ALU.mult, [b_Wx, b_tab], [b_Mt2], "pool")
                TTd(Sprev[:, 0], Mt1[:], Mt2[:], ALU.subtract, [b_Mt1, b_Mt2], [b_Sprev])
                TTd(Mt1[:], wr_, sinD, ALU.mult, [b_Wx, b_tab, b_Sprev], [b_Mt1]); TTd(Mt2[:], wi_, cosD, ALU.mult, [b_Wx, b_tab, b_Sprev], [b_Mt2], "pool")
                TTd(Sprev[:, 1], Mt1[:], Mt2[:], ALU.add, [b_Mt1, b_Mt2], [b_Sprev])
                ygv = ygT[:, t4, :].rearrange("p (k j) -> p j k", j=TCH)
                for j in range(8):
                    ps, bps = pp.get()
                    nmm = (j + 1) + 8
                    i = 0
                    for tau in range(j + 1):
                        kb.op("pe", lambda e, ps=ps, tau=tau, j=j, i=i, nmm=nmm, uv=uv: e.matmul(ps[:, 0:NCH], lhsT=Kfir[:, tau, :], rhs=uv[:, j - tau, :],
                                                                                         start=(i == 0), stop=(i == nmm - 1)),
                              reads=[b_Kfir, b_u], writes=[bps])
                        i += 1
                    for ns in range(4):
                        for ri in range(2):
                            kb.op("pe", lambda e, ps=ps, ns=ns, ri=ri, j=j, i=i, nmm=nmm: e.matmul(ps[:, 0:NCH], lhsT=Wout[:, j, ns * 2 + ri, :], rhs=Sprev[:, ri, ns, :],
                                                                                             start=(i == 0), stop=(i == nmm - 1)),
                                  reads=[b_Wout, b_Sprev], writes=[bps])
                            i += 1
                    kb.op("act", lambda e, ps=ps, j=j, ygv=ygv: e.activation(out=ygv[:, j, :], in_=ps[:, 0:NCH], func=AF.Gelu), reads=[bps], writes=[b_ygT])

    if DEBUG:
        kb.dma("sp", dbg_yg, ygT[:], reads=[b_ygT], writes=[b_out])
    sigs = [(sq[:, i, :], b_sq) for i in range(8)]
    for tt in range(4):
        tsl_ = slice(tt * TT, (tt + 1) * TT)
        sgs = []
        for m in range(4):
            ps, bps = pp.get()
            for c in range(4):
                kb.op("pe", lambda e, ps=ps, m=m, c=c, tsl_=tsl_: e.matmul(ps[:, :], lhsT=w_glu[:, c, m * 128:(m + 1) * 128], rhs=ygT[:, c, tsl_], start=(c == 0), stop=(c == 3)),
                      reads=[b_wreg, b_ygT], writes=[bps])
            sg_, b_sg_ = sigs[(tt * 4 + m) % 8]
            kb.op("act", lambda e, ps=ps, sg_=sg_: e.activation(out=sg_, in_=ps[:, :], func=AF.Sigmoid), reads=[bps], writes=[b_sg_])
            sgs.append((sg_, b_sg_))
        for m in range(4):
            sg_, b_sg_ = sgs[m]
            kb.op("dve", lambda e, m=m, sg_=sg_, tsl_=tsl_: e.tensor_tensor(out=ygT[:, m, tsl_], in0=ygT[:, m, tsl_], in1=sg_, op=ALU.mult),
                  reads=[b_ygT, b_sg_], writes=[b_ygT])

    if DEBUG:
        kb.dma("sp", dbg_ssm, ygT[:], reads=[b_ygT], writes=[b_out])
        kb.dma("sp", dbg_q, qT[:], reads=[b_qT], writes=[b_out])
        kb.dma("sp", dbg_k, kT[:], reads=[b_kT], writes=[b_out])
        kb.dma("sp", dbg_v, vtm[:], reads=[b_vtm], writes=[b_out])
    attn, b_attn = V("attn", [128, 4, TT], BF16)
    mix = ht
    pT = [V(f"pT{i}", [128, 256], BF16) for i in range(6)]
    npt = 0
    den_s, b_den_s = rstd, b_rstd
    ag8, b_ag8 = ag, b_ag
    epsf = eps_t[:].rearrange("p a n k -> p (a n k)")
    acc_banks = []
    for i in range(4):
        bb = B(f"accbank{i}")
        bb.w = b_eps_t.w
        bb.readers = list(b_eps_t.readers)
        acc_banks.append((epsf[:, i * 512:(i + 1) * 512], bb))
    for tt in range(4):
        for tq in range(4):
            kvh = tq // 2
            pso, bpso = acc_banks[(tq % 2) * 2]
            psd, bpsd = acc_banks[(tq % 2) * 2 + 1]
            for hh in range(2):
                prow = slice(64 * hh, 64 * hh + 64)
                ptiles = {}
                for kbi in range(4 * tt - 1, 4 * tt + 4):
                    qb0 = kbi
                    pst, bpst = pp.get()
                    q_lo = max(qb0, 4 * tt) * 128
                    q_hi = min(qb0 + 2, 4 * tt + 4) * 128
                    ncol = q_hi - q_lo
                    moff = (q_lo - qb0 * 128)
                    kcols = slice((kbi + 1) * 128, (kbi + 2) * 128)
                    kb.op("pe", lambda e, pst=pst, prow=prow, kcols=kcols, q_lo=q_lo, q_hi=q_hi, ncol=ncol, tq=tq, kvh=kvh:
                          e.matmul(pst[:, 0:ncol], lhsT=kT[prow, kvh, kcols], rhs=qT[prow, tq, q_lo:q_hi], start=True, stop=False),
                          reads=[b_kT, b_qT], writes=[bpst])
                    kb.op("pe", lambda e, pst=pst, ncol=ncol, moff=moff: e.matmul(pst[:, 0:ncol], lhsT=ident[:], rhs=maskneg[:, moff:moff + ncol], start=False, stop=True),
                          reads=[b_ident, b_maskneg], writes=[bpst])
                    pt_, b_pt_ = pT[npt % 6]
                    npt += 1
                    kb.op("act", lambda e, pst=pst, pt_=pt_, ncol=ncol: e.activation(out=pt_[:, 0:ncol], in_=pst[:, 0:ncol], func=AF.Exp, scale=0.125),
                          reads=[bpst], writes=[b_pt_])
                    ptiles[kbi] = (pt_, b_pt_, q_lo)
                for qb in range(4 * tt, 4 * tt + 4):
                    ocol = slice((qb - 4 * tt) * 128, (qb - 4 * tt + 1) * 128)
                    for which, kbi in enumerate((qb - 1, qb)):
                        pt_, b_pt_, q_lo = ptiles[kbi]
                        c0 = qb * 128 - q_lo
                        vblk = kbi + 1
                        ones_l = onesfl if kbi == -1 else ones_bf
                        b_ones_l = b_onesfl if kbi == -1 else b_ones_bf
                        kb.op("pe", lambda e, pso=pso, prow=prow, ocol=ocol, pt_=pt_, c0=c0, vblk=vblk, which=which, kvh=kvh:
                              e.matmul(pso[prow, ocol], lhsT=vtm[:, vblk, kvh * 64:(kvh + 1) * 64], rhs=pt_[:, c0:c0 + 128], start=(which == 0), stop=(which == 1)),
                              reads=[b_vtm, b_pt_], writes=[bpso])
                        kb.op("pe", lambda e, psd=psd, prow=prow, ocol=ocol, pt_=pt_, c0=c0, which=which, ones_l=ones_l:
                              e.matmul(psd[prow, ocol], lhsT=ones_l[:, 0:64], rhs=pt_[:, c0:c0 + 128], start=(which == 0), stop=(which == 1)),
                              reads=[b_ones_l, b_pt_], writes=[bpsd])
            kb.op("dve", lambda e, psd=psd, tq=tq: e.tensor_scalar(out=den_s[:], in0=psd[:, :], scalar1=esnk[:, tq:tq + 1], scalar2=None, op0=ALU.add),
                  reads=[bpsd, b_esnk], writes=[b_den_s])
            kb.op("dve", lambda e: e.reciprocal(out=den_s[:], in_=den_s[:]), reads=[b_den_s], writes=[b_den_s])
            kb.op("dve", lambda e, pso=pso, tq=tq: e.tensor_tensor(out=attn[:, tq, :], in0=pso[:, :], in1=den_s[:], op=ALU.mult),
                  reads=[bpso, b_den_s], writes=[b_attn])
        if DEBUG:
            kb.dma("sp", dbg_attn[:, :, tt * TT:(tt + 1) * TT], attn[:], reads=[b_attn], writes=[b_out])
        mx, b_mx = mix[tt % 2]
        tsl_ = slice(tt * TT, (tt + 1) * TT)
        rms_tile(kb, pp, C, attn, b_attn, ag, b_ag, mx[:, 0:4, :], b_mx, 4, TT, sq, b_sq, rstd, b_rstd, 512)
        rms_tile(kb, pp, C, ygT[:, :, tsl_], b_ygT, sg, b_sg, mx[:, 4:8, :], b_mx, 4, TT, sq, b_sq, rstd, b_rstd, 512)
        x_t, b_x = xt[0]
        kb.dma("sp", x_t[:], xT_all[:, :, NPRE + tt * TT:NPRE + (tt + 1) * TT], reads=[dram_in], writes=[b_x])
        for m in range(8):
            ps, bps = pp.get()
            for c in range(8):
                kb.op("pe", lambda e, ps=ps, m=m, c=c, mx=mx: e.matmul(ps[:, :], lhsT=w_o[:, c, m * 128:(m + 1) * 128], rhs=mx[:, c, :], start=(c == 0), stop=(c == 7)),
                      reads=[b_wreg, b_mx], writes=[bps])
            kb.op("dve", lambda e, ps=ps, m=m, x_t=x_t: e.tensor_tensor(out=x_t[:, m, :], in0=x_t[:, m, :], in1=ps[:, :], op=ALU.add), reads=[bps, b_x], writes=[b_x])
        kb.dma("sp", xT_out[:, :, tsl_], x_t[:], reads=[b_x], writes=[b_out])
    kb.emit(final_groups=["out"])
    kb.close()
    return nc


NCORES = 8


def _fm(a):
    T, Dd = a.shape
    return np.ascontiguousarray(a.reshape(T, Dd // 128, 128).transpose(2, 1, 0))


def _fm_inv(a):
    P, Cc, T = a.shape
    return np.ascontiguousarray(a.transpose(2, 1, 0).reshape(T, Cc * P))


def _pc(v, nchunk):
    return np.ascontiguousarray(np.asarray(v, np.float32).reshape(nchunk, 128).T)


def _nlayout(a):
    a = np.asarray(a, np.float32)
    rest = a.shape[2:]
    a = a.reshape(4, 8, 4, 16, *rest)
    a = np.moveaxis(a, (1, 3), (0, 1))
    return np.ascontiguousarray(a.reshape(128, 16, *rest))


def mixer_weight_inputs(inp, l):
    w_in = np.asarray(inp["w_in"][l], np.float32)
    q = w_in[:, 0:512]; k = w_in[:, 512:640]; v = w_in[:, 640:768]; u = w_in[:, 768:1280]
    w_arr = np.ascontiguousarray(np.concatenate([q, k[:, 0:64], k[:, 0:64], k[:, 64:128], k[:, 64:128], v, u], axis=1))
    d = {
        "g1": _pc(inp["attn_norm_g"][l], 8),
        "w_in": w_arr,
        "qg": np.ascontiguousarray(np.tile(np.asarray(inp["q_norm_g"][l], np.float32), 2).reshape(128, 1)),
        "kg": np.ascontiguousarray(np.tile(np.asarray(inp["k_norm_g"][l], np.float32), 2).reshape(128, 1)),
        "snk": np.ascontiguousarray(np.repeat(np.asarray(inp["sinks"][l], np.float32).reshape(4, 2), 64, axis=1).T),
        "lamre": _nlayout(inp["lam_re"][l]), "lamim": _nlayout(inp["lam_im"][l]),
        "ldt": _nlayout(np.repeat(np.asarray(inp["log_dt"][l], np.float32)[:, None], 64, axis=1)),
        "bre": _nlayout(inp["b_re"][l]), "bim": _nlayout(inp["b_im"][l]),
        "cre": _nlayout(np.swapaxes(np.asarray(inp["c_re"][l]), 1, 2)), "cim": _nlayout(np.swapaxes(np.asarray(inp["c_im"][l]), 1, 2)),
        "dsk": _pc(inp["d_skip"][l], 4),
        "w_glu": np.ascontiguousarray(np.asarray(inp["w_glu"][l], np.float32)),
        "ag": _pc(inp["attn_out_g"][l], 4), "sg": _pc(inp["ssm_out_g"][l], 4),
        "w_o": np.ascontiguousarray(np.asarray(inp["w_o"][l], np.float32)),
    }
    return d


def mixer_in_maps(inp, l, x_full):
    wd = mixer_weight_inputs(inp, l)
    maps = []
    for core in range(NCORES):
        b, q = divmod(core, 4)
        own_end = (q + 1) * NTOK
        xs = x_full[b, 0:own_end]
        pad = NPRE + NTOK - own_end
        if pad:
            xs = np.concatenate([np.zeros((pad, DM), np.float32), xs], axis=0)
        m = dict(wd)
        m["xT_all"] = _fm(xs)
        m["flag"] = np.full((128, 1), 0.0 if q == 0 else 1.0, np.float32)
        maps.append(m)
    return maps


def gather_x(results, key="xT_out"):
    out = np.zeros((2, 4 * NTOK, DM), np.float32)
    for core in range(NCORES):
        b, q = divmod(core, 4)
        out[b, q * NTOK:(q + 1) * NTOK] = _fm_inv(results[core][key])
    return out


DFF = 3584
FB = 512
NFB = DFF // FB


def build_ffn(n_exp):
    nc = bass.Bass("TRN2", target_bir_lowering=False)

    def D(name, shape, dt=F32, kind="ExternalInput"):
        return nc.dram_tensor(name, list(shape), dt, kind=kind).ap()
    xT_in = D("xT_in", [128, 8, NTOK]); g2_d = D("g2", [128, 8])
    wg_d = D("wg", [n_exp, DM, DFF]); wu_d = D("wu", [n_exp, DM, DFF]); wd_d = D("wd", [n_exp, DFF, DM])
    moe = n_exp > 1
    if moe:
        rw_d = D("rw", [128, 8, 8])
    xT_out = D("xT_out", [128, 8, NTOK], kind="ExternalOutput")
    kb = KB(nc)
    B = kb.buf
    C = make_consts(kb, full=False)
    pp = PsumPool(kb, 8)
    V = lambda name, shape, dt=F32: (kb.sb(shape, dt, name=name), B(name))
    dram_in = B("dram_in"); b_out = B("xT_out", grp="out")
    ones_bf, b_ones_bf = C["ones_bf"]; ident, b_ident = C["ident"]
    g2, b_g2 = V("g2s", [128, 8]); b_g2.grp = "const"
    kb.dma("sp", g2[:], g2_d, reads=[dram_in], writes=[b_g2])
    xT, _ = V("xT", [128, 8, NTOK])
    b_xT = [B(f"xT{t}") for t in range(4)]
    hT, _ = V("hT", [128, 8, NTOK], BF16)
    b_hT = [B(f"hT{t}") for t in range(4)]
    sq, b_sq = V("sq", [128, 8, TT], BF16); rstd, b_rstd = V("rstd", [128, TT])
    wbuf = [(V(f"wg{i}", [128, 8, FB], BF16), V(f"wu{i}", [128, 8, FB], BF16), V(f"wd{i}", [128, FB // 128, DM], BF16)) for i in range(2)]
    Abuf = [V(f"A{i}", [128, FB // 128, TT], BF16) for i in range(2)]
    stmp = [V(f"stmp{i}", [128, TT], BF16) for i in range(3)]

    def load_w(e, fb, i):
        (wg, b_wg), (wu, b_wu), (wd, b_wd) = wbuf[i]
        fsl = slice(fb * FB, (fb + 1) * FB)
        kb.dma("pool", wg[:], wg_d[e, :, fsl].rearrange("(c p) f -> p c f", p=128), reads=[dram_in], writes=[b_wg])
        kb.dma("pool", wu[:], wu_d[e, :, fsl].rearrange("(c p) f -> p c f", p=128), reads=[dram_in], writes=[b_wu])
        kb.dma("pool", wd[:], wd_d[e, fsl, :].rearrange("(t p) d -> p t d", p=128), reads=[dram_in], writes=[b_wd])

    blocks = [(e, fb) for e in range(n_exp) for fb in range(NFB)]
    load_w(blocks[0][0], blocks[0][1], 0)
    for tt in range(4):
        tsl = slice(tt * TT, (tt + 1) * TT)
        kb.dma("sp", xT[:, :, tsl], xT_in[:, :, tsl], reads=[dram_in], writes=[b_xT[tt]])
        rms_tile(kb, pp, C, xT[:, :, tsl], b_xT[tt], g2, b_g2, hT[:, :, tsl], b_hT[tt], 8, TT, sq, b_sq, rstd, b_rstd, DM)

    if moe:
        rw, b_rw = V("rws", [128, 8, 8]); b_rw.grp = "const"
        kb.dma("sp", rw[:], rw_d, reads=[dram_in], writes=[b_rw])
        gate_bc, _ = V("gate_bc", [128, 8, NTOK], BF16)
        b_gate = [B(f"gate{t}") for t in range(4)]
        hn32, b_hn32 = V("hn32", [128, 8, 128])
        lg, b_lg = V("lg", [128, 16, 8])
        for hb in range(16):
            tt = hb // 4
            hsl = slice(hb * 128, (hb + 1) * 128)
            ps, bps = pp.get()
            for c in range(8):
                kb.op("act", lambda e, c=c, hsl=hsl: e.activation(out=sq[:, c, 0:128], in_=xT[:, c, hsl], func=AF.Square), reads=[b_xT[tt]], writes=[b_sq])
            for c in range(8):
                kb.op("pe", lambda e, c=c, ps=ps: e.matmul(ps[:, 0:128], lhsT=ones_bf[:], rhs=sq[:, c, 0:128], start=(c == 0), stop=(c == 7)),
                      reads=[b_ones_bf, b_sq], writes=[bps])
            kb.op("act", lambda e, ps=ps: e.activation(out=rstd[:, 0:128], in_=ps[:, 0:128], func=AF.Sqrt, scale=1.0 / DM, bias=C["eps"][0][:, 0:1]),
                  reads=[bps, C["eps"][1]], writes=[b_rstd])
            kb.op("dve", lambda e: e.reciprocal(out=rstd[:, 0:128], in_=rstd[:, 0:128]), reads=[b_rstd], writes=[b_rstd])
            for c in range(8):
                kb.op("dve", lambda e, c=c, hsl=hsl: e.scalar_tensor_tensor(out=hn32[:, c, :], in0=xT[:, c, hsl], scalar=g2[:, c:c + 1], in1=rstd[:, 0:128],
                                                                          op0=ALU.mult, op1=ALU.mult), reads=[b_xT[tt], b_g2, b_rstd], writes=[b_hn32])
            for bl in range(1):
                ps2, bps2 = pp.get()
                for c in range(8):
                    kb.op("pe", lambda e, c=c, ps2=ps2, bl=bl: e.matmul(ps2[:, 0:8], lhsT=hn32[:, c, bl * 128:(bl + 1) * 128], rhs=rw[:, c, :], start=(c == 0), stop=(c == 7)),
                          reads=[b_hn32, b_rw], writes=[bps2])
                kb.op("act", lambda e, ps2=ps2, hb=hb, bl=bl: e.activation(out=lg[:, hb, :], in_=ps2[:, 0:8], func=AF.Copy), reads=[bps2], writes=[b_lg])
        m1, b_m1 = V("m1", [128, 16]); m2, b_m2 = V("m2", [128, 16]); eq1, b_eq1 = V("eq1", [128, 16, 8]); eq2, b_eq2 = V("eq2", [128, 16, 8])
        lg2, b_lg2 = V("lg2", [128, 16, 8]); gg, b_gg = V("gg", [128, 3, 16]); gate_tm, b_gate_tm = V("gate_tm", [128, 16, 8])
        bc8 = lambda t: t[:].unsqueeze(2).to_broadcast([128, 16, 8])
        kb.op("dve", lambda e: e.tensor_reduce(out=m1[:], in_=lg[:], axis=AX.X, op=ALU.max), reads=[b_lg], writes=[b_m1])
        kb.op("dve", lambda e: e.tensor_tensor(out=eq1[:], in0=lg[:], in1=bc8(m1), op=ALU.is_equal), reads=[b_lg, b_m1], writes=[b_eq1])
        kb.op("dve", lambda e: e.scalar_tensor_tensor(out=lg2[:], in0=eq1[:], scalar=-1e30, in1=lg[:], op0=ALU.mult, op1=ALU.add), reads=[b_eq1, b_lg], writes=[b_lg2])
        kb.op("dve", lambda e: e.tensor_reduce(out=m2[:], in_=lg2[:], axis=AX.X, op=ALU.max), reads=[b_lg2], writes=[b_m2])
        kb.op("dve", lambda e: e.tensor_tensor(out=eq2[:], in0=lg2[:], in1=bc8(m2), op=ALU.is_equal), reads=[b_lg2, b_m2], writes=[b_eq2])
        kb.op("dve", lambda e: e.tensor_tensor(out=gg[:, 0], in0=m2[:], in1=m1[:], op=ALU.subtract), reads=[b_m1, b_m2], writes=[b_gg])
        kb.op("act", lambda e: e.activation(out=gg[:, 0], in_=gg[:, 0], func=AF.Exp), reads=[b_gg], writes=[b_gg])
        kb.op("dve", lambda e: e.tensor_scalar(out=gg[:, 1], in0=gg[:, 0], scalar1=1.0, scalar2=None, op0=ALU.add), reads=[b_gg], writes=[b_gg])
        kb.op("dve", lambda e: e.reciprocal(out=gg[:, 1], in_=gg[:, 1]), reads=[b_gg], writes=[b_gg])
        kb.op("dve", lambda e: e.tensor_tensor(out=gg[:, 2], in0=gg[:, 0], in1=gg[:, 1], op=ALU.mult), reads=[b_gg], writes=[b_gg])
        kb.op("dve", lambda e: e.tensor_tensor(out=gate_tm[:], in0=eq1[:], in1=gg[:, 1].unsqueeze(2).to_broadcast([128, 16, 8]), op=ALU.mult),
              reads=[b_eq1, b_gg], writes=[b_gate_tm])
        kb.op("dve", lambda e: e.tensor_tensor(out=eq2[:], in0=eq2[:], in1=gg[:, 2].unsqueeze(2).to_broadcast([128, 16, 8]), op=ALU.mult),
              reads=[b_eq2, b_gg], writes=[b_eq2])
        kb.op("dve", lambda e: e.tensor_tensor(out=gate_tm[:], in0=gate_tm[:], in1=eq2[:], op=ALU.add), reads=[b_gate_tm, b_eq2], writes=[b_gate_tm])
        dg = [V(f"dg{i}", [128, 128], BF16) for i in range(4)]
        ndg = 0
        for blk in range(16):
            tt = blk // 4
            for e_ in range(8):
                dgt, b_dgt = dg[ndg % 4]
                ndg += 1
                kb.op("dve", lambda e, dgt=dgt, blk=blk, e_=e_: e.tensor_scalar(out=dgt[:], in0=ident[:], scalar1=gate_tm[:, blk, e_:e_ + 1], scalar2=None, op0=ALU.mult),
                      reads=[b_ident, b_gate_tm], writes=[b_dgt])
                ps, bps = pp.get()
                kb.op("pe", lambda e, ps=ps, dgt=dgt: e.matmul(ps[:, 0:128], lhsT=ones_bf[:], rhs=dgt[:], start=True, stop=True), reads=[b_ones_bf, b_dgt], writes=[bps])
                kb.op("act", lambda e, ps=ps, blk=blk, e_=e_: e.activation(out=gate_bc[:, e_, blk * 128:(blk + 1) * 128], in_=ps[:, 0:128], func=AF.Copy),
                      reads=[bps], writes=[b_gate[tt]])

    nA = 0
    for bi, (e_, fb) in enumerate(blocks):
        i = bi % 2
        if bi + 1 < len(blocks):
            load_w(blocks[bi + 1][0], blocks[bi + 1][1], (bi + 1) % 2)
        (wg, b_wg), (wu, b_wu), (wd, b_wd) = wbuf[i]
        for tt in range(4):
            tsl = slice(tt * TT, (tt + 1) * TT)
            A, b_A = Abuf[nA % 2]
            nA += 1
            for ft in range(FB // 128):
                psg, bpsg = pp.get()
                psu, bpsu = pp.get()
                for c in range(8):
                    kb.op("pe", lambda e, psg=psg, c=c, ft=ft, wg=wg, tsl=tsl: e.matmul(psg[:, :], lhsT=wg[:, c, ft * 128:(ft + 1) * 128], rhs=hT[:, c, tsl], start=(c == 0), stop=(c == 7)),
                          reads=[b_wg, b_hT[tt]], writes=[bpsg])
                for c in range(8):
                    kb.op("pe", lambda e, psu=psu, c=c, ft=ft, wu=wu, tsl=tsl: e.matmul(psu[:, :], lhsT=wu[:, c, ft * 128:(ft + 1) * 128], rhs=hT[:, c, tsl], start=(c == 0), stop=(c == 7)),
                          reads=[b_wu, b_hT[tt]], writes=[bpsu])
                st, b_st = stmp[(nA * 4 + ft) % 3]
                kb.op("act", lambda e, psg=psg, st=st: e.activation(out=st[:], in_=psg[:, :], func=AF.Silu), reads=[bpsg], writes=[b_st])
                if moe:
                    kb.op("pool", lambda e, st=st, e_=e_, tsl=tsl: e.tensor_tensor(out=st[:], in0=st[:], in1=gate_bc[:, e_, tsl], op=ALU.mult),
                          reads=[b_st, b_gate[tt]], writes=[b_st])
                kb.op("dve", lambda e, psu=psu, st=st, A=A, ft=ft: e.tensor_tensor(out=A[:, ft, :], in0=st[:], in1=psu[:, :], op=ALU.mult),
                      reads=[b_st, bpsu], writes=[b_A])
            for dtile in range(8):
                pso, bpso = pp.get()
                for ft in range(FB // 128):
                    kb.op("pe", lambda e, pso=pso, ft=ft, dtile=dtile, wd=wd, A=A: e.matmul(pso[:, :], lhsT=wd[:, ft, dtile * 128:(dtile + 1) * 128], rhs=A[:, ft, :],
                                                                                     start=(ft == 0), stop=(ft == FB // 128 - 1)),
                          reads=[b_wd, b_A], writes=[bpso])
                kb.op("dve", lambda e, pso=pso, dtile=dtile, tsl=tsl: e.tensor_tensor(out=xT[:, dtile, tsl], in0=xT[:, dtile, tsl], in1=pso[:, :], op=ALU.add),
                      reads=[bpso, b_xT[tt]], writes=[b_xT[tt]])
    for tt in range(4):
        tsl = slice(tt * TT, (tt + 1) * TT)
        kb.dma("sp", xT_out[:, :, tsl], xT[:, :, tsl], reads=[b_xT[tt]], writes=[b_out])
    kb.emit(final_groups=["out"])
    kb.close()
    return nc


from concourse.bass_utils import run_bass_kernel_spmd

_PROGS = {}


def _prog(name):
    if name not in _PROGS:
        if name == "mixer":
            _PROGS[name] = build_mixer()
        elif name == "dense":
            _PROGS[name] = build_ffn(1)
        else:
            _PROGS[name] = build_ffn(8)
    return _PROGS[name]


def _run(nc, maps):
    res = run_bass_kernel_spmd(nc, maps, core_ids=list(range(NCORES)))
    return res.results


def _ffn_maps(inp, x_full, l):
    maps = []
    for core in range(NCORES):
        b, q = divmod(core, 4)
        m = {"xT_in": _fm(x_full[b, q * NTOK:(q + 1) * NTOK]), "g2": _pc(inp["ffn_norm_g"][l], 8)}
        if l % 2 == 0:
            i = l // 2
            m.update(wg=np.ascontiguousarray(inp["dense_wg"][i:i + 1], dtype=np.float32), wu=np.ascontiguousarray(inp["dense_wu"][i:i + 1], dtype=np.float32),
                     wd=np.ascontiguousarray(inp["dense_wd"][i:i + 1], dtype=np.float32))
        else:
            i = l // 2
            m.update(wg=np.ascontiguousarray(inp["moe_wg"][i], dtype=np.float32), wu=np.ascontiguousarray(inp["moe_wu"][i], dtype=np.float32),
                     wd=np.ascontiguousarray(inp["moe_wd"][i], dtype=np.float32),
                     rw=np.ascontiguousarray(np.asarray(inp["router_w"][i], np.float32).reshape(8, 128, 8).transpose(1, 0, 2)))
        maps.append(m)
    return maps


def kernel(**inputs):
    inp = {k: np.asarray(v) for k, v in inputs.items()}
    x = np.ascontiguousarray(inp["x"], dtype=np.float32)
    for l in range(2):
        x = gather_x(_run(_prog("mixer"), mixer_in_maps(inp, l, x)))
        x = gather_x(_run(_prog("dense" if l % 2 == 0 else "moe"), _ffn_maps(inp, x, l)))
    return x.astype(np.float32)
```
